# Optimizing a Trainium2 kernel written in Bass

```python
import math
import jax, jax.numpy as jnp
from jax import lax
import numpy as np

D_MODEL = 4096
BATCH = 2
SEQ = 4096
DEPTH = 2

DIFF_QK_DIM = 64
DIFF_V_DIM = 2 * DIFF_QK_DIM
N_DIFF_HEADS = D_MODEL // (2 * DIFF_V_DIM)
MOBA_HEAD_DIM = 128
N_MOBA_HEADS = D_MODEL // (2 * MOBA_HEAD_DIM)
MOBA_BLOCK = 256
MOBA_TOPK = 3
MOBA_Q_CHUNK = 32
DENSE_Q_BLOCK = 128
A_QK_W = N_DIFF_HEADS * 2 * DIFF_QK_DIM
A_V_W = N_DIFF_HEADS * DIFF_V_DIM
B_W = N_MOBA_HEADS * MOBA_HEAD_DIM
EVEN_SPLITS = [int(v) for v in np.cumsum([A_QK_W, A_QK_W, A_V_W, B_W, B_W])]

SWA_HEAD_DIM = 64
N_SWA_HEADS = D_MODEL // SWA_HEAD_DIM
N_SWA_KV_HEADS = N_SWA_HEADS // 8
SWA_WINDOW = 128
ODD_SPLITS = [N_SWA_HEADS * SWA_HEAD_DIM, (N_SWA_HEADS + N_SWA_KV_HEADS) * SWA_HEAD_DIM]

N_EXPERTS = 32
TOP_K = 4
D_EXPERT = 1024
SWIGLU_LIMIT = 7.0
SWIGLU_ALPHA = 1.702

LN_EPS = 1e-5
SUBLN_EPS = 1e-5
DEEPNORM_ALPHA = (2.0 * DEPTH) ** 0.25
DEEPNORM_BETA = (8.0 * DEPTH) ** -0.25

kernel_name = "hybrid_diff_moba_swa_moe_deepnorm"


def alibi_slopes(n_heads):
    return 2.0 ** (-8.0 * (jnp.arange(n_heads, dtype=jnp.float32) + 1.0) / n_heads)


def layer_norm(x, g, b):
    xf = x.astype(jnp.float32)
    mu = jnp.mean(xf, axis=-1, keepdims=True)
    var = jnp.mean(jnp.square(xf - mu), axis=-1, keepdims=True)
    y = (xf - mu) * lax.rsqrt(var + LN_EPS) * g.astype(jnp.float32) + b.astype(jnp.float32)
    return y.astype(x.dtype)


def post_norm(x, h, g, b):
    return layer_norm(DEEPNORM_ALPHA * x + h, g, b)


def diff_attention(q, k, v, lam, lam_init, subln_g, slopes):
    B_, H, _, S_, dq = q.shape
    nq = S_ // DENSE_Q_BLOCK
    qb = q.reshape(B_, H, 2, nq, DENSE_Q_BLOCK, dq).transpose(3, 0, 1, 2, 4, 5)
    kpos = jnp.arange(S_)
    scale = DIFF_QK_DIM ** -0.5

    def one_block(args):
        qc, blk = args
        qpos = blk * DENSE_Q_BLOCK + jnp.arange(DENSE_Q_BLOCK)
        dist = (qpos[:, None] - kpos[None, :]).astype(jnp.float32)
        bias = -slopes[:, None, None] * dist
        s = jnp.einsum('bhmqd,bhmkd->bhmqk', qc, k).astype(jnp.float32) * scale + bias[None, :, None]
        s = jnp.where(dist >= 0, s, -jnp.inf)
        p = jax.nn.softmax(s, axis=-1)
        a = p[:, :, 0] - lam * p[:, :, 1]
        return jnp.einsum('bhqk,bhkd->bhqd', a.astype(v.dtype), v)

    o = lax.map(one_block, (qb, jnp.arange(nq)))
    o = o.transpose(1, 0, 3, 2, 4).reshape(B_, S_, H, DIFF_V_DIM)
    of = o.astype(jnp.float32)
    of = of * lax.rsqrt(jnp.mean(jnp.square(of), axis=-1, keepdims=True) + SUBLN_EPS)
    of = of * subln_g.astype(jnp.float32) * (1.0 - lam_init)
    return of.astype(v.dtype).reshape(B_, S_, H * DIFF_V_DIM)


def moba_attention(q, k, v, slopes):
    B_, H, S_, Dh = q.shape
    nb = -(-S_ // MOBA_BLOCK)
    Sp = nb * MOBA_BLOCK
    pad = ((0, 0), (0, 0), (0, Sp - S_), (0, 0))
    q, k, v = jnp.pad(q, pad), jnp.pad(k, pad), jnp.pad(v, pad)
    kb = k.reshape(B_, H, nb, MOBA_BLOCK, Dh)
    vb = v.reshape(B_, H, nb, MOBA_BLOCK, Dh)
    k_mean = jnp.mean(kb.astype(jnp.float32), axis=3)
    gate = jnp.einsum('bhsd,bhnd->bhsn', q.astype(jnp.float32), k_mean)
    qblk = jnp.arange(Sp) // MOBA_BLOCK
    past = jnp.arange(nb)[None, :] < qblk[:, None]
    gate = jnp.where(past, gate, -jnp.inf)
    k_sel = min(MOBA_TOPK, nb)
    _, sel = lax.top_k(gate, k_sel)
    own = jnp.broadcast_to(qblk[None, None, :, None], (B_, H, Sp, 1))
    idx = jnp.concatenate([sel, own], axis=-1)
    valid = jnp.concatenate([sel < qblk[None, None, :, None], jnp.ones_like(own, dtype=bool)], axis=-1)
    ns = k_sel + 1
    nc = Sp // MOBA_Q_CHUNK
    qc = q.reshape(B_, H, nc, MOBA_Q_CHUNK, Dh).transpose(2, 0, 1, 3, 4)
    idx_c = idx.reshape(B_, H, nc, MOBA_Q_CHUNK, ns).transpose(2, 0, 1, 3, 4)
    val_c = valid.reshape(B_, H, nc, MOBA_Q_CHUNK, ns).transpose(2, 0, 1, 3, 4)
    bi = jnp.arange(B_)[:, None, None, None]
    hi = jnp.arange(H)[None, :, None, None]
    offs = jnp.arange(MOBA_BLOCK)
    scale = Dh ** -0.5

    def one_chunk(args):
        qx, ix, vx, c = args
        kg = kb[bi, hi, ix]
        vg = vb[bi, hi, ix]
        qpos = c * MOBA_Q_CHUNK + jnp.arange(MOBA_Q_CHUNK)
        kpos = ix[..., None] * MOBA_BLOCK + offs
        dist = (qpos[None, None, :, None, None] - kpos).astype(jnp.float32)
        ok = vx[..., None] & (dist >= 0)
        s = jnp.einsum('bhqd,bhqnkd->bhqnk', qx, kg).astype(jnp.float32) * scale
        s = s - slopes[None, :, None, None, None] * dist
        s = jnp.where(ok, s, -jnp.inf)
        p = jax.nn.softmax(s.reshape(B_, H, MOBA_Q_CHUNK, -1), axis=-1).reshape(s.shape)
        return jnp.einsum('bhqnk,bhqnkd->bhqd', p.astype(vg.dtype), vg)

    o = lax.map(one_chunk, (qc, idx_c, val_c, jnp.arange(nc)))
    o = o.transpose(1, 0, 3, 2, 4).reshape(B_, Sp, H * Dh)
    return o[:, :S_]


def swa_sink_attention(q, k, v, sinks, slopes):
    B_, S_, Hq, Dh = q.shape
    Hkv = k.shape[2]
    G = Hq // Hkv
    W = SWA_WINDOW
    nb = S_ // W
    qb = q.reshape(B_, nb, W, Hkv, G, Dh)
    kb = k.reshape(B_, nb, W, Hkv, Dh)
    vb = v.reshape(B_, nb, W, Hkv, Dh)
    prev = lambda t: jnp.pad(t, ((0, 0), (1, 0), (0, 0), (0, 0), (0, 0)))[:, :-1]
    kcat = jnp.concatenate([prev(kb), kb], axis=2)
    vcat = jnp.concatenate([prev(vb), vb], axis=2)
    qpos = jnp.arange(nb)[:, None] * W + jnp.arange(W)[None, :]
    kpos = (jnp.arange(nb)[:, None] - 1) * W + jnp.arange(2 * W)[None, :]
    dist_i = qpos[:, :, None] - kpos[:, None, :]
    ok = (dist_i >= 0) & (dist_i < W) & (kpos[:, None, :] >= 0)
    dist = dist_i.astype(jnp.float32)
    s = jnp.einsum('bnqhgd,bnkhd->bnhgqk', qb, kcat).astype(jnp.float32) * (Dh ** -0.5)
    s = s - slopes.reshape(Hkv, G)[None, None, :, :, None, None] * dist[None, :, None, None]
    s = jnp.where(ok[None, :, None, None], s, -jnp.inf)
    sink = sinks.astype(jnp.float32).reshape(Hkv, G)[None, None, :, :, None, None]
    m = jnp.maximum(jnp.max(s, axis=-1, keepdims=True), sink)
    e = jnp.exp(s - m)
    p = e / (jnp.sum(e, axis=-1, keepdims=True) + jnp.exp(sink - m))
    o = jnp.einsum('bnhgqk,bnkhd->bnqhgd', p.astype(vcat.dtype), vcat)
    return o.reshape(B_, S_, Hq * Dh)


def diff_moba_mixer(x, w_in, lq1, lk1, lq2, lk2, subln_g, w_o, layer_idx):
    B_, S_, _ = x.shape
    proj = x @ w_in
    a_q, a_k, a_v, b_q, b_k, b_v = jnp.split(proj, EVEN_SPLITS, axis=-1)
    to_diff = lambda t: t.reshape(B_, S_, N_DIFF_HEADS, 2, DIFF_QK_DIM).transpose(0, 2, 3, 1, 4)
    to_heads = lambda t, h, d: t.reshape(B_, S_, h, d).transpose(0, 2, 1, 3)
    lam_init = 0.8 - 0.6 * math.exp(-0.3 * layer_idx)
    lam = (jnp.exp(jnp.sum(lq1.astype(jnp.float32) * lk1.astype(jnp.float32)))
           - jnp.exp(jnp.sum(lq2.astype(jnp.float32) * lk2.astype(jnp.float32))) + lam_init)
    slopes = alibi_slopes(N_DIFF_HEADS + N_MOBA_HEADS)
    out_a = diff_attention(to_diff(a_q), to_diff(a_k), to_heads(a_v, N_DIFF_HEADS, DIFF_V_DIM),
                           lam, lam_init, subln_g, slopes[0::2])
    out_b = moba_attention(to_heads(b_q, N_MOBA_HEADS, MOBA_HEAD_DIM),
                           to_heads(b_k, N_MOBA_HEADS, MOBA_HEAD_DIM),
                           to_heads(b_v, N_MOBA_HEADS, MOBA_HEAD_DIM), slopes[1::2])
    return jnp.concatenate([out_a, out_b], axis=-1) @ w_o


def swa_mixer(x, w_in, sinks, w_o):
    B_, S_, _ = x.shape
    q, k, v = jnp.split(x @ w_in, ODD_SPLITS, axis=-1)
    q = q.reshape(B_, S_, N_SWA_HEADS, SWA_HEAD_DIM)
    k = k.reshape(B_, S_, N_SWA_KV_HEADS, SWA_HEAD_DIM)
    v = v.reshape(B_, S_, N_SWA_KV_HEADS, SWA_HEAD_DIM)
    return swa_sink_attention(q, k, v, sinks, alibi_slopes(N_SWA_HEADS)) @ w_o


def moe_ffn(x, router_w, router_b, w_gate, b_gate, w_up, b_up, w_down, b_down):
    B_, S_, D = x.shape
    xt = x.reshape(-1, D)
    logits = (xt @ router_w).astype(jnp.float32) + router_b.astype(jnp.float32)
    top_v, top_i = lax.top_k(logits, TOP_K)
    top_w = jax.nn.softmax(top_v, axis=-1)
    combine = jnp.sum(jax.nn.one_hot(top_i, N_EXPERTS, dtype=jnp.float32) * top_w[..., None], axis=1)
    combine = combine.astype(x.dtype)
    y = jnp.zeros_like(xt)
    for e in range(N_EXPERTS):
        gate = jnp.minimum(xt @ w_gate[e] + b_gate[e], SWIGLU_LIMIT)
        up = jnp.clip(xt @ w_up[e] + b_up[e], -SWIGLU_LIMIT, SWIGLU_LIMIT)
        h = (up + 1.0) * gate * jax.nn.sigmoid(SWIGLU_ALPHA * gate)
        y = y + combine[:, e:e + 1] * (h @ w_down[e] + b_down[e])
    return y.reshape(B_, S_, D)


def setup_inputs(seed: int = 0) -> dict:
    key = jax.random.key(seed)
    ks = list(jax.random.split(key, 64))
    cnt = [0]

    def nrm(shape, scale):
        k = ks[cnt[0]]
        cnt[0] += 1
        return jax.random.normal(k, shape, jnp.float32) * scale

    d = D_MODEL
    s_in = d ** -0.5
    beta = DEEPNORM_BETA

    def moe_params():
        return (nrm((d, N_EXPERTS), s_in), nrm((N_EXPERTS,), 0.01),
                nrm((N_EXPERTS, d, D_EXPERT), s_in), nrm((N_EXPERTS, D_EXPERT), 0.02),
                nrm((N_EXPERTS, d, D_EXPERT), s_in), nrm((N_EXPERTS, D_EXPERT), 0.02),
                nrm((N_EXPERTS, D_EXPERT, d), D_EXPERT ** -0.5 * beta), nrm((N_EXPERTS, d), 0.02))

    def ln_params():
        return 1.0 + nrm((d,), 0.02), nrm((d,), 0.02)

    x = nrm((BATCH, SEQ, d), 1.0)
    l0_w_in = jnp.concatenate([nrm((d, A_QK_W), s_in), nrm((d, A_QK_W), s_in), nrm((d, A_V_W), s_in * beta),
                               nrm((d, B_W), s_in), nrm((d, B_W), s_in), nrm((d, B_W), s_in * beta)], axis=1)
    l0_lq1, l0_lk1, l0_lq2, l0_lk2 = [nrm((DIFF_QK_DIM,), 0.1) for _ in range(4)]
    l0_subln_g = 1.0 + nrm((DIFF_V_DIM,), 0.02)
    l0_w_o = nrm((A_V_W + B_W, d), (A_V_W + B_W) ** -0.5 * beta)
    l0_ln1_g, l0_ln1_b = ln_params()
    l0_moe = moe_params()
    l0_ln2_g, l0_ln2_b = ln_params()
    kv_w = N_SWA_KV_HEADS * SWA_HEAD_DIM
    q_w = N_SWA_HEADS * SWA_HEAD_DIM
    l1_w_in = jnp.concatenate([nrm((d, q_w), s_in), nrm((d, kv_w), s_in), nrm((d, kv_w), s_in * beta)], axis=1)
    l1_sinks = nrm((N_SWA_HEADS,), 0.5)
    l1_w_o = nrm((q_w, d), q_w ** -0.5 * beta)
    l1_ln1_g, l1_ln1_b = ln_params()
    l1_moe = moe_params()
    l1_ln2_g, l1_ln2_b = ln_params()
    return {
        "x": x,
        "l0_w_in": l0_w_in, "l0_lambda_q1": l0_lq1, "l0_lambda_k1": l0_lk1,
        "l0_lambda_q2": l0_lq2, "l0_lambda_k2": l0_lk2, "l0_subln_g": l0_subln_g, "l0_w_o": l0_w_o,
        "l0_ln1_g": l0_ln1_g, "l0_ln1_b": l0_ln1_b,
        "l0_router_w": l0_moe[0], "l0_router_b": l0_moe[1], "l0_w_gate": l0_moe[2], "l0_b_gate": l0_moe[3],
        "l0_w_up": l0_moe[4], "l0_b_up": l0_moe[5], "l0_w_down": l0_moe[6], "l0_b_down": l0_moe[7],
        "l0_ln2_g": l0_ln2_g, "l0_ln2_b": l0_ln2_b,
        "l1_w_in": l1_w_in, "l1_sinks": l1_sinks, "l1_w_o": l1_w_o,
        "l1_ln1_g": l1_ln1_g, "l1_ln1_b": l1_ln1_b,
        "l1_router_w": l1_moe[0], "l1_router_b": l1_moe[1], "l1_w_gate": l1_moe[2], "l1_b_gate": l1_moe[3],
        "l1_w_up": l1_moe[4], "l1_b_up": l1_moe[5], "l1_w_down": l1_moe[6], "l1_b_down": l1_moe[7],
        "l1_ln2_g": l1_ln2_g, "l1_ln2_b": l1_ln2_b,
    }


def reference(x, l0_w_in, l0_lambda_q1, l0_lambda_k1, l0_lambda_q2, l0_lambda_k2, l0_subln_g, l0_w_o,
              l0_ln1_g, l0_ln1_b, l0_router_w, l0_router_b, l0_w_gate, l0_b_gate, l0_w_up, l0_b_up,
              l0_w_down, l0_b_down, l0_ln2_g, l0_ln2_b,
              l1_w_in, l1_sinks, l1_w_o, l1_ln1_g, l1_ln1_b, l1_router_w, l1_router_b, l1_w_gate,
              l1_b_gate, l1_w_up, l1_b_up, l1_w_down, l1_b_down, l1_ln2_g, l1_ln2_b):
    mixer_args = (
        (l0_w_in, l0_lambda_q1, l0_lambda_k1, l0_lambda_q2, l0_lambda_k2, l0_subln_g, l0_w_o),
        (l1_w_in, l1_sinks, l1_w_o),
    )
    norm_args = (
        (l0_ln1_g, l0_ln1_b, l0_ln2_g, l0_ln2_b),
        (l1_ln1_g, l1_ln1_b, l1_ln2_g, l1_ln2_b),
    )
    moe_args = (
        (l0_router_w, l0_router_b, l0_w_gate, l0_b_gate, l0_w_up, l0_b_up, l0_w_down, l0_b_down),
        (l1_router_w, l1_router_b, l1_w_gate, l1_b_gate, l1_w_up, l1_b_up, l1_w_down, l1_b_down),
    )
    for layer in range(DEPTH):
        if layer % 2 == 0:
            h = diff_moba_mixer(x, *mixer_args[layer], layer_idx=layer)
        else:
            h = swa_mixer(x, *mixer_args[layer])
        g1, b1, g2, b2 = norm_args[layer]
        x = post_norm(x, h, g1, b1)
        x = post_norm(x, moe_ffn(x, *moe_args[layer]), g2, b2)
    return x
```

```python
import math
from contextlib import ExitStack

import numpy as np
import ml_dtypes
import concourse.bass as bass
import concourse.mybir as mybir
from concourse.bass_utils import run_bass_kernel_spmd

F32 = mybir.dt.float32
BF16 = mybir.dt.bfloat16
ALU = mybir.AluOpType
AF = mybir.ActivationFunctionType
AX = mybir.AxisListType

S = 4096
D = 4096
NCORES = 2
DEPTH = 2
ALPHA = (2.0 * DEPTH) ** 0.25
LN_EPS = 1e-5
NEG = -1.0e30
NE = 32
ROT_THRESH = 12000
DBG_HEADS = None
DBG_QBS = None
DBG_TB = None
DBG_NE = None
DBG_VIRT = False
DBG_G = None
DH = 1024


def slopes(n):
    return [2.0 ** (-8.0 * (i + 1.0) / n) for i in range(n)]


class Buf:
    __slots__ = ("name", "wr", "rd", "nowaw", "ep", "sticky")

    def __init__(self, name, nowaw=False, sticky=False):
        self.name = name
        self.wr = {}
        self.rd = {}
        self.nowaw = nowaw
        self.ep = 0
        self.sticky = sticky


class T:
    __slots__ = ("t", "b")

    def __init__(self, t, b):
        self.t = t
        self.b = b


class KB:
    def __init__(self, nc, es):
        self.nc = nc
        self.es = es
        self.engs = {"pe": nc.tensor, "dve": nc.vector, "act": nc.scalar, "pool": nc.gpsimd, "sp": nc.sync}
        self.sems = {}
        self.cnt = {}
        self.waited = {e: {} for e in self.engs}
        self.uid = 0
        self.epoch = 0
        self.gen = {}
        self.nobar = set()
        self.log = {e: [] for e in self.engs}
        for e in self.engs:
            self.sem("e_" + e)

    def sem(self, key):
        if key not in self.sems:
            self.sems[key] = self.es.enter_context(self.nc.semaphore("s_" + key))
            self.cnt[key] = 0
        return self.sems[key]

    def _uname(self, es, name):
        live = self.__dict__.setdefault("live", set())
        nm = name
        i = 0
        while nm in live:
            i += 1
            nm = f"{name}_{i}"
        live.add(nm)
        es.callback(live.discard, nm)
        return nm

    def tile(self, es, name, shape, dt):
        self.uid += 1
        nm = self._uname(es, name)
        return T(es.enter_context(self.nc.sbuf_tensor(f"{nm}_u{self.uid}", shape, dt)), Buf(nm))

    def ptile(self, es, name, shape, dt):
        self.uid += 1
        nm = self._uname(es, "P" + name)
        return T(es.enter_context(self.nc.psum_tensor(f"{nm}_u{self.uid}", shape, dt)), Buf(nm))

    def _fresh(self, bufs):
        for b in bufs:
            if b.sticky:
                continue
            if b.ep != self.epoch:
                b.wr = {}
                b.rd = {}
                b.ep = self.epoch

    def _waits(self, eng, reads, writes):
        self._fresh(reads)
        self._fresh(writes)
        need = {}

        def add(dep):
            for kk, v in dep.items():
                if need.get(kk, 0) < v:
                    need[kk] = v

        for b in reads:
            add(b.wr)
        for b in writes:
            if not b.nowaw:
                add(b.wr)
            add(b.rd)
        w = self.waited[eng]
        for kk, v in need.items():
            if w.get(kk, 0) >= v:
                continue
            if kk == "e_" + eng and (eng == "pe" or v > self.cnt[kk]):
                continue
            w[kk] = v
            self.engs[eng].wait_ge(self.sems[kk], v)
            self.log[eng].append(("w", (kk, self.gen.get(kk, 0)), v))

    def op(self, eng, fn, reads=(), writes=(), inc=True):
        self._waits(eng, reads, writes)
        key = "e_" + eng
        ins = fn(self.engs[eng])
        v = self.cnt[key] + 1
        if inc:
            ins.then_inc(self.sems[key], 1)
            self.cnt[key] = v
            self.log[eng].append(("i", (key, self.gen.get(key, 0)), 1))
        for b in reads:
            if b.rd.get(key, 0) < v:
                b.rd[key] = v
        for b in writes:
            if b.nowaw:
                b.wr[key] = v
            else:
                b.wr = {key: v}
                b.rd = {}
        return ins

    def dma(self, eng, pairs, reads, writes, kbuf):
        self._waits(eng, reads, writes)
        key = "d_" + kbuf.name
        self.sem(key)
        for (o, i) in pairs:
            self.engs[eng].dma_start(out=o, in_=i).then_inc(self.sems[key], 16)
            self.cnt[key] += 16
            self.log[eng].append(("i", (key, self.gen.get(key, 0)), 16))
        v = self.cnt[key]
        for b in reads:
            if b.rd.get(key, 0) < v:
                b.rd[key] = v
        for b in writes:
            if b.nowaw:
                b.wr[key] = v
            else:
                b.wr = {key: v}
                b.rd = {}

    def barrier(self):
        for eng in self.engs:
            w = self.waited[eng]
            for kk, v in self.cnt.items():
                if v == 0 or w.get(kk, 0) >= v or kk in self.nobar:
                    continue
                if kk == "e_" + eng:
                    continue
                w[kk] = v
                self.engs[eng].wait_ge(self.sems[kk], v)
                self.log[eng].append(("w", (kk, self.gen.get(kk, 0)), v))
        self.epoch += 1
        for kk, v in list(self.cnt.items()):
            if v > ROT_THRESH and kk not in self.nobar:
                g = self.gen.get(kk, 0) + 1
                self.gen[kk] = g
                self.sems[kk] = self.es.enter_context(self.nc.semaphore(f"s_{kk}_g{g}"))
                self.cnt[kk] = 0
                for e in self.engs:
                    self.waited[e].pop(kk, None)

    def final_wait(self):
        w = self.waited["sp"]
        for kk, v in self.cnt.items():
            if v == 0 or w.get(kk, 0) >= v or kk == "e_sp":
                continue
            w[kk] = v
            self.engs["sp"].wait_ge(self.sems[kk], v)
            self.log["sp"].append(("w", (kk, self.gen.get(kk, 0)), v))


def stage_inproj(k, xT, xT_b, w, w_b, blocks, PT, PT_b, VN, VN_b):
    TBS = 1024
    with ExitStack() as es:
        xTb = k.tile(es, "xTb", [128, 32, TBS], BF16)
        Wb = [k.tile(es, "Wb", [128, 32, 512], BF16) for _ in range(2)]
        ev = [k.tile(es, "ev", [128, 512], BF16) for _ in range(4)]
        ps = [k.ptile(es, "psA", [128, 512], F32) for _ in range(4)]
        xTv = xT.rearrange("(kc p) s -> p kc s", p=128)
        wv = w.rearrange("(kc p) n -> p kc n", p=128)
        cnt = 0
        wi = 0
        for tb in range(DBG_TB or (S // TBS)):
            k.dma("pool", [(xTb.t[:, j * 8:(j + 1) * 8, :], xTv[:, j * 8:(j + 1) * 8, tb * TBS:(tb + 1) * TBS])
                           for j in range(4)], reads=[xT_b], writes=[xTb.b], kbuf=xTb.b)
            for (c0, kind, dst0, scale) in blocks:
                W = Wb[wi % 2]
                wi += 1
                k.dma("pool", [(W.t[:, j * 8:(j + 1) * 8, :], wv[:, j * 8:(j + 1) * 8, c0:c0 + 512])
                               for j in range(4)], reads=[w_b], writes=[W.b], kbuf=W.b)
                if kind == "T":
                    for ct in range(4):
                        for th in range(TBS // 512):
                            p = ps[cnt % 4]
                            e = ev[cnt % 4]
                            cnt += 1
                            for kc in range(32):
                                k.op("pe", lambda en: en.matmul(p.t[:], W.t[:, kc, ct * 128:(ct + 1) * 128],
                                                                xTb.t[:, kc, th * 512:(th + 1) * 512],
                                                                start=(kc == 0), stop=(kc == 31)),
                                     reads=[W.b, xTb.b], writes=[p.b], inc=(kc == 31))
                            k.op("act", lambda en: en.activation(out=e.t[:], in_=p.t[:], func=AF.Copy, scale=float(scale)),
                                 reads=[p.b], writes=[e.b])
                            t0 = tb * TBS + th * 512
                            k.dma("sp", [(PT[dst0 + ct * 128:dst0 + (ct + 1) * 128, t0:t0 + 512], e.t[:])],
                                  reads=[e.b], writes=[PT_b], kbuf=e.b)
                else:
                    for tt in range(TBS // 128):
                        p = ps[cnt % 4]
                        e = ev[cnt % 4]
                        cnt += 1
                        for kc in range(32):
                            k.op("pe", lambda en: en.matmul(p.t[:], xTb.t[:, kc, tt * 128:(tt + 1) * 128], W.t[:, kc, :],
                                                            start=(kc == 0), stop=(kc == 31)),
                                 reads=[W.b, xTb.b], writes=[p.b], inc=(kc == 31))
                        k.op("dve", lambda en: en.tensor_copy(e.t[:], p.t[:]), reads=[p.b], writes=[e.b])
                        t0 = tb * TBS + tt * 128
                        k.dma("sp", [(VN[t0:t0 + 128, dst0:dst0 + 512], e.t[:])], reads=[e.b], writes=[VN_b], kbuf=e.b)
    k.barrier()


def attn_qblock(k, qb, kts, maps, QTh, KTh_ap, Vaug, VW, Bh, slope, ps_s, sT, pT, ps_o, rot, pv_ok,
                selT=None, onehot=None, qcols=None):
    q0 = qb * 512 if qcols is None else qcols
    for m, (p0, p1) in enumerate(maps):
        for half in range(2):
            k.op("dve", lambda en: en.memset(ps_o[m][half].t[:], 0.0), writes=[ps_o[m][half].b])
        for kt in kts:
            i = rot[0] % 3
            rot[0] += 1
            P = ps_s[i]
            k.op("pe", lambda en: en.matmul(P.t[:], KTh_ap(p0, p1, kt), QTh.t[p0:p1, q0:q0 + 512],
                                            start=True, stop=(selT is None)),
                 reads=[Vaug.b, QTh.b], writes=[P.b], inc=(selT is None))
            if selT is not None:
                kb = kt // 2
                k.op("pe", lambda en: en.matmul(P.t[:], onehot.t[0:16, kb * 128:(kb + 1) * 128],
                                                selT.t[0:16, qb * 512:(qb + 1) * 512], start=False, stop=True),
                     reads=[onehot.b, selT.b], writes=[P.b])
            o = 512 * qb - 128 * kt
            if o <= 128:
                c0 = o + 384
                cb = 0.0
            else:
                c0 = 512
                cb = -slope * (o - 128)
            if cb == 0.0:
                k.op("dve", lambda en: en.tensor_tensor(out=sT[i].t[:], in0=P.t[:], in1=Bh.t[:, c0:c0 + 512], op=ALU.add),
                     reads=[P.b, Bh.b], writes=[sT[i].b])
            else:
                k.op("dve", lambda en: en.scalar_tensor_tensor(out=sT[i].t[:], in0=P.t[:], scalar=float(cb),
                                                               in1=Bh.t[:, c0:c0 + 512], op0=ALU.add, op1=ALU.add),
                     reads=[P.b, Bh.b], writes=[sT[i].b])
            k.op("act", lambda en: en.activation(out=pT[i].t[:], in_=sT[i].t[:], func=AF.Exp),
                 reads=[sT[i].b], writes=[pT[i].b])
            qts = [qt for qt in range(4) if pv_ok(qb, qt, kt)]
            for j, qt in enumerate(qts):
                bank = ps_o[m][qt // 2]
                off = (qt % 2) * 256
                k.op("pe", lambda en: en.matmul(bank.t[:, off:off + VW], pT[i].t[:, qt * 128:(qt + 1) * 128],
                                                Vaug.t[:, kt, 0:VW], start=False, stop=False, skip_group_check=True),
                     reads=[pT[i].b, Vaug.b], writes=[bank.b], inc=(j == len(qts) - 1))


def stage_attn_l0(k, C, PT, PT_b, VN, VN_b, AOT, AOT_b, lam_in, subln_g_in, cin_b):
    sl = slopes(32)
    with ExitStack() as es:
        dtab = k.tile(es, "dtab", [128, 1024], F32)
        mcau = k.tile(es, "mcau", [128, 1024], F32)
        ident = k.tile(es, "identb", [128, 128], BF16)
        onehot = k.tile(es, "oh16", [16, 2048], BF16)
        pastb = k.tile(es, "pastb", [128, 512], F32)
        pastm = k.tile(es, "pastm", [128, 512], F32)
        ownm = k.tile(es, "ownm", [128, 512], F32)
        for tl, nm in [(dtab, "dtab"), (mcau, "mcausal"), (ident, "ident_bf"), (onehot, "onehot16"),
                       (pastb, "pastbias"), (pastm, "pastmask"), (ownm, "ownmask")]:
            k.dma("sp", [(tl.t[:], C[nm][:, :])], reads=[cin_b], writes=[tl.b], kbuf=tl.b)
        lv = k.tile(es, "lv", [128, 4, 64], F32)
        sm0 = k.tile(es, "sm0", [128, 8], F32)
        gB = k.tile(es, "gB", [128, 128], F32)
        k.dma("sp", [(lv.t[:, j, :], lam_in[j][None, :].to_broadcast([128, 64])) for j in range(4)],
              reads=[cin_b], writes=[lv.b], kbuf=lv.b)
        k.dma("sp", [(gB.t[:], subln_g_in[None, :].to_broadcast([128, 128]))], reads=[cin_b], writes=[gB.b], kbuf=gB.b)
        lam_init = 0.8 - 0.6 * math.exp(-0.3 * 0)
        k.op("dve", lambda en: en.tensor_tensor(out=lv.t[:, 0, :], in0=lv.t[:, 0, :], in1=lv.t[:, 1, :], op=ALU.mult),
             reads=[lv.b], writes=[lv.b])
        k.op("dve", lambda en: en.tensor_tensor(out=lv.t[:, 2, :], in0=lv.t[:, 2, :], in1=lv.t[:, 3, :], op=ALU.mult),
             reads=[lv.b], writes=[lv.b])
        k.op("dve", lambda en: en.reduce_sum(out=sm0.t[:, 0:1], in_=lv.t[:, 0, :], axis=AX.X), reads=[lv.b], writes=[sm0.b])
        k.op("dve", lambda en: en.reduce_sum(out=sm0.t[:, 1:2], in_=lv.t[:, 2, :], axis=AX.X), reads=[lv.b], writes=[sm0.b])
        k.op("act", lambda en: en.activation(out=sm0.t[:, 2:4], in_=sm0.t[:, 0:2], func=AF.Exp), reads=[sm0.b], writes=[sm0.b])
        k.op("dve", lambda en: en.tensor_tensor(out=sm0.t[:, 4:5], in0=sm0.t[:, 3:4], in1=sm0.t[:, 2:3], op=ALU.subtract),
             reads=[sm0.b], writes=[sm0.b])
        k.op("dve", lambda en: en.tensor_scalar(out=sm0.t[:, 5:6], in0=sm0.t[:, 4:5], scalar1=-lam_init, scalar2=None, op0=ALU.add),
             reads=[sm0.b], writes=[sm0.b])
        k.op("dve", lambda en: en.tensor_scalar(out=gB.t[:], in0=gB.t[:], scalar1=1.0 - lam_init, scalar2=None, op0=ALU.mult),
             reads=[gB.b], writes=[gB.b])
        neglam = sm0.t[:, 5:6]

        QTh = [k.tile(es, "QTh", [128, S], BF16) for _ in range(2)]
        KTh = [k.tile(es, "KTh", [128, 16, 256], BF16) for _ in range(2)]
        Vaug = [k.tile(es, "Vaug", [128, 32, 132], BF16) for _ in range(2)]
        Bh = [k.tile(es, "Bh", [128, 1024], F32) for _ in range(2)]
        for v in Vaug:
            k.op("pool", lambda en: en.memset(v.t[:, :, 128:132], 1.0), writes=[v.b])
        selT = k.tile(es, "selT", [16, S], BF16)
        ps_s = [k.ptile(es, "ps_s", [128, 512], F32) for _ in range(3)]
        sT = [k.tile(es, "sT", [128, 512], F32) for _ in range(3)]
        pT = [k.tile(es, "pT", [128, 512], BF16) for _ in range(3)]
        ps_o = [[k.ptile(es, "ps_o", [128, 512], F32) for _ in range(2)] for _ in range(2)]
        ps_t = k.ptile(es, "ps_t", [128, 1024], BF16)
        sm = k.tile(es, "sm", [128, 8], F32)
        o1 = k.tile(es, "o1", [128, 128], F32)
        o2 = k.tile(es, "o2", [128, 128], F32)
        ofs = [k.tile(es, "of", [128, 128], BF16) for _ in range(4)]
        aoT = [k.tile(es, "aoT", [128, 512], BF16) for _ in range(2)]
        km = k.tile(es, "km", [128, 16], F32)
        kmb = k.tile(es, "kmb", [128, 16], BF16)
        G = k.tile(es, "G", [128, 512], F32)
        top8 = k.tile(es, "top8", [128, 256], F32)
        selm = k.tile(es, "selm", [128, 512], F32)
        sbb = k.tile(es, "sbb", [128, 512], BF16)
        rot = [0]
        ao_i = 0
        VNv = VN.rearrange("(kt p) c -> p kt c", p=128)

        def pv_ok(qb, qt, kt):
            return kt <= 4 * qb + qt

        for hi_, h in enumerate(DBG_HEADS if DBG_HEADS is not None else range(32)):
            if hi_ > 0 and hi_ % 4 == 0:
                k.barrier()
            hs = h % 2
            is_diff = h < 16
            hh = h if is_diff else h - 16
            slope = sl[2 * hh] if is_diff else sl[2 * hh + 1]
            qrow = (0 if is_diff else 4096) + hh * 128
            krow = (2048 if is_diff else 6144) + hh * 128
            vcol = (0 if is_diff else 2048) + hh * 128
            Q, Kt, V, B = QTh[hs], KTh[hs], Vaug[hs], Bh[hs]
            k.dma("sp", [(Q.t[:], PT[qrow:qrow + 128, :])], reads=[PT_b], writes=[Q.b], kbuf=Q.b)
            k.dma("sp", [(Kt.t[:, :, :], PT[krow:krow + 128, :].rearrange("p (a b) -> p a b", b=256))],
                  reads=[PT_b], writes=[Kt.b, V.b], kbuf=Kt.b)
            k.dma("sp", [(V.t[:, :, 0:128], VNv[:, :, vcol:vcol + 128])], reads=[VN_b, Kt.b], writes=[V.b], kbuf=V.b)
            k.op("dve", lambda en: en.scalar_tensor_tensor(out=B.t[:], in0=dtab.t[:], scalar=float(-slope), in1=mcau.t[:],
                                                           op0=ALU.mult, op1=ALU.add),
                 reads=[dtab.b, mcau.b], writes=[B.b])

            def KT_ap(p0, p1, kt, Kt=Kt):
                return Kt.t[p0:p1, kt // 2, (kt % 2) * 128:(kt % 2 + 1) * 128]

            if not is_diff:
                k.op("dve", lambda en: en.tensor_reduce(out=km.t[:], in_=Kt.t[:, :, :], axis=AX.X, op=ALU.add),
                     reads=[V.b], writes=[km.b])
                k.op("dve", lambda en: en.tensor_scalar(out=kmb.t[:], in0=km.t[:], scalar1=1.0 / 256.0, scalar2=None, op0=ALU.mult),
                     reads=[km.b], writes=[kmb.b])
                pg = ps_s[0]
                for t in range(32):
                    k.op("pe", lambda en: en.matmul(pg.t[:, t * 16:(t + 1) * 16], Q.t[:, t * 128:(t + 1) * 128], kmb.t[:, :],
                                                    start=True, stop=True),
                         reads=[Q.b, kmb.b], writes=[pg.b], inc=(t == 31))
                k.op("dve", lambda en: en.tensor_tensor(out=G.t[:], in0=pg.t[:], in1=pastb.t[:], op=ALU.add),
                     reads=[pg.b, pastb.b], writes=[G.b])
                for t in range(32):
                    k.op("dve", lambda en: en.max(out=top8.t[:, t * 8:(t + 1) * 8], in_=G.t[:, t * 16:(t + 1) * 16]),
                         reads=[G.b], writes=[top8.b], inc=(t == 31))
                for t in range(32):
                    k.op("dve", lambda en: en.tensor_scalar(out=selm.t[:, t * 16:(t + 1) * 16], in0=G.t[:, t * 16:(t + 1) * 16],
                                                            scalar1=top8.t[:, t * 8 + 2:t * 8 + 3], scalar2=None, op0=ALU.is_ge),
                         reads=[G.b, top8.b], writes=[selm.b], inc=(t == 31))
                k.op("dve", lambda en: en.tensor_tensor(out=selm.t[:], in0=selm.t[:], in1=pastm.t[:], op=ALU.mult),
                     reads=[selm.b, pastm.b], writes=[selm.b])
                k.op("dve", lambda en: en.tensor_tensor(out=selm.t[:], in0=selm.t[:], in1=ownm.t[:], op=ALU.add),
                     reads=[selm.b, ownm.b], writes=[selm.b])
                k.op("dve", lambda en: en.tensor_scalar(out=sbb.t[:], in0=selm.t[:], scalar1=-1.0, scalar2=30000.0,
                                                        op0=ALU.add, op1=ALU.mult),
                     reads=[selm.b], writes=[sbb.b])
                for g in range(4):
                    for t8 in range(8):
                        t = g * 8 + t8
                        k.op("pe", lambda en: en.transpose(out=ps_t.t[0:16, (t8 % 4) * 128:(t8 % 4 + 1) * 128],
                                                           in_=sbb.t[:, t * 16:(t + 1) * 16], identity=ident.t[:]),
                             reads=[sbb.b, ident.b], writes=[ps_t.b], inc=(t8 % 4 == 3))
                        if t8 % 4 == 3:
                            c0 = (t - 3) * 128
                            k.op("act", lambda en: en.activation(out=selT.t[0:16, c0:c0 + 512], in_=ps_t.t[0:16, 0:512], func=AF.Copy),
                                 reads=[ps_t.b], writes=[selT.b])

            maps = [(0, 64), (64, 128)] if is_diff else [(0, 128)]
            for qb in (DBG_QBS if DBG_QBS is not None else range(8)):
                attn_qblock(k, qb, list(range(4 * qb + 4)), maps, Q, KT_ap, V, 129, B, slope, ps_s, sT, pT, ps_o, rot, pv_ok,
                            selT=None if is_diff else selT, onehot=None if is_diff else onehot)
                ao = aoT[ao_i % 2]
                ao_i += 1
                for qt in range(4):
                    bank0 = ps_o[0][qt // 2]
                    off = (qt % 2) * 256
                    of = ofs[qt]
                    k.op("dve", lambda en: en.reciprocal(sm.t[:, 0:1], bank0.t[:, off + 128:off + 129]),
                         reads=[bank0.b], writes=[sm.b])
                    if is_diff:
                        bank1 = ps_o[1][qt // 2]
                        k.op("dve", lambda en: en.reciprocal(sm.t[:, 1:2], bank1.t[:, off + 128:off + 129]),
                             reads=[bank1.b], writes=[sm.b])
                        k.op("dve", lambda en: en.tensor_tensor(out=sm.t[:, 2:3], in0=sm.t[:, 1:2], in1=neglam, op=ALU.mult),
                             reads=[sm.b, sm0.b], writes=[sm.b])
                        k.op("dve", lambda en: en.tensor_scalar(out=o1.t[:], in0=bank0.t[:, off:off + 128], scalar1=sm.t[:, 0:1],
                                                                scalar2=None, op0=ALU.mult),
                             reads=[bank0.b, sm.b], writes=[o1.b])
                        k.op("dve", lambda en: en.scalar_tensor_tensor(out=o1.t[:], in0=bank1.t[:, off:off + 128], scalar=sm.t[:, 2:3],
                                                                       in1=o1.t[:], op0=ALU.mult, op1=ALU.add),
                             reads=[bank1.b, sm.b, o1.b], writes=[o1.b])
                        k.op("dve", lambda en: en.tensor_tensor(out=o2.t[:], in0=o1.t[:], in1=o1.t[:], op=ALU.mult),
                             reads=[o1.b], writes=[o2.b])
                        k.op("dve", lambda en: en.reduce_sum(out=sm.t[:, 3:4], in_=o2.t[:], axis=AX.X), reads=[o2.b], writes=[sm.b])
                        k.op("dve", lambda en: en.tensor_scalar(out=sm.t[:, 4:5], in0=sm.t[:, 3:4], scalar1=1.0 / 128.0, scalar2=1e-5,
                                                                op0=ALU.mult, op1=ALU.add), reads=[sm.b], writes=[sm.b])
                        k.op("act", lambda en: en.activation(out=sm.t[:, 5:6], in_=sm.t[:, 4:5], func=AF.Ln), reads=[sm.b], writes=[sm.b])
                        k.op("act", lambda en: en.activation(out=sm.t[:, 6:7], in_=sm.t[:, 5:6], func=AF.Exp, scale=-0.5),
                             reads=[sm.b], writes=[sm.b])
                        k.op("dve", lambda en: en.scalar_tensor_tensor(out=of.t[:], in0=o1.t[:], scalar=sm.t[:, 6:7], in1=gB.t[:],
                                                                       op0=ALU.mult, op1=ALU.mult),
                             reads=[o1.b, sm.b, gB.b], writes=[of.b])
                    else:
                        k.op("dve", lambda en: en.tensor_scalar(out=of.t[:], in0=bank0.t[:, off:off + 128], scalar1=sm.t[:, 0:1],
                                                                scalar2=None, op0=ALU.mult),
                             reads=[bank0.b, sm.b], writes=[of.b])
                    k.op("pe", lambda en: en.transpose(out=ps_t.t[:, qt * 128:(qt + 1) * 128], in_=of.t[:], identity=ident.t[:]),
                         reads=[of.b, ident.b], writes=[ps_t.b], inc=(qt == 3))
                k.op("act", lambda en: en.activation(out=ao.t[:], in_=ps_t.t[:, 0:512], func=AF.Copy), reads=[ps_t.b], writes=[ao.b])
                arow = (0 if is_diff else 2048) + hh * 128
                k.dma("sp", [(AOT[arow:arow + 128, qb * 512:(qb + 1) * 512], ao.t[:])], reads=[ao.b], writes=[AOT_b], kbuf=ao.b)
    k.barrier()


def stage_attn_l1(k, C, PT, PT_b, VN, VN_b, AOT, AOT_b, sinks_in, cin_b):
    sl = slopes(64)
    with ExitStack() as es:
        dtab = k.tile(es, "dtab", [128, 1024], F32)
        mband = k.tile(es, "mband", [128, 1024], F32)
        ident = k.tile(es, "identb", [128, 128], BF16)
        for tl, nm in [(dtab, "dtab"), (mband, "mband"), (ident, "ident_bf")]:
            k.dma("sp", [(tl.t[:], C[nm][:, :])], reads=[cin_b], writes=[tl.b], kbuf=tl.b)
        snk = k.tile(es, "snk", [128, 64], F32)
        k.dma("sp", [(snk.t[:], sinks_in[None, :].to_broadcast([128, 64]))], reads=[cin_b], writes=[snk.b], kbuf=snk.b)
        k.op("act", lambda en: en.activation(out=snk.t[:], in_=snk.t[:], func=AF.Exp), reads=[snk.b], writes=[snk.b])
        QTp = [k.tile(es, "QTp", [128, S], BF16) for _ in range(2)]
        KT2 = [k.tile(es, "KT2", [128, S], BF16) for _ in range(2)]
        Vaug = [k.tile(es, "Vaug1", [128, 32, 68], BF16) for _ in range(2)]
        Bh = [k.tile(es, "Bh1", [128, 1024], F32) for _ in range(2)]
        for v in Vaug:
            k.op("pool", lambda en: en.memset(v.t[:, :, 64:68], 1.0), writes=[v.b])
        ps_s = [k.ptile(es, "ps_s", [128, 512], F32) for _ in range(3)]
        sT = [k.tile(es, "sT", [128, 512], F32) for _ in range(3)]
        pT = [k.tile(es, "pT", [128, 512], BF16) for _ in range(3)]
        ps_o = [[k.ptile(es, "ps_o", [128, 512], F32) for _ in range(2)] for _ in range(2)]
        ps_t = k.ptile(es, "ps_t", [128, 1024], BF16)
        sm = k.tile(es, "sm", [128, 8], F32)
        ofs = [k.tile(es, "of", [128, 128], BF16) for _ in range(4)]
        aoT = [k.tile(es, "aoT", [128, 512], BF16) for _ in range(2)]
        rot = [0]
        ao_i = 0
        bi = 0
        VNv = VN.rearrange("(kt p) c -> p kt c", p=128)

        def pv_ok(qb, qt, kt):
            return kt == 4 * qb + qt or kt == 4 * qb + qt - 1

        for g in range(DBG_G or 8):
            if g > 0:
                k.barrier()
            K2 = KT2[g % 2]
            V = Vaug[g % 2]
            k.dma("sp", [(K2.t[0:64, :], PT[4096 + g * 64:4096 + (g + 1) * 64, :]),
                         (K2.t[64:128, :], PT[4096 + g * 64:4096 + (g + 1) * 64, :])],
                  reads=[PT_b], writes=[K2.b, V.b], kbuf=K2.b)
            k.dma("sp", [(V.t[:, :, 0:64], VNv[:, :, g * 64:(g + 1) * 64])], reads=[VN_b, K2.b], writes=[V.b], kbuf=V.b)

            def KT_ap(p0, p1, kt, K2=K2):
                return K2.t[p0:p1, kt * 128:(kt + 1) * 128]

            for pr in range(1 if DBG_G else 4):
                hp = g * 4 + pr
                Q = QTp[hp % 2]
                k.dma("sp", [(Q.t[:], PT[hp * 128:(hp + 1) * 128, :])], reads=[PT_b], writes=[Q.b], kbuf=Q.b)
                Bs = []
                for j in range(2):
                    B = Bh[bi % 2]
                    bi += 1
                    hq = 2 * hp + j
                    k.op("dve", lambda en: en.scalar_tensor_tensor(out=B.t[:], in0=dtab.t[:], scalar=float(-sl[hq]), in1=mband.t[:],
                                                                   op0=ALU.mult, op1=ALU.add),
                         reads=[dtab.b, mband.b], writes=[B.b])
                    Bs.append(B)
                for qb in (DBG_QBS if DBG_QBS is not None else range(8)):
                    kts = list(range(max(0, 4 * qb - 1), 4 * qb + 4))
                    for j in range(2):
                        attn_qblock(k, qb, kts, [(64 * j, 64 * j + 64)], Q, KT_ap, V, 65, Bs[j], sl[2 * hp + j],
                                    ps_s, sT, pT, [ps_o[j]], rot, pv_ok)
                    ao = aoT[ao_i % 2]
                    ao_i += 1
                    for qt in range(4):
                        off = (qt % 2) * 256
                        of = ofs[qt]
                        for j in range(2):
                            bank = ps_o[j][qt // 2]
                            hq = 2 * hp + j
                            k.op("dve", lambda en: en.tensor_tensor(out=sm.t[:, j:j + 1], in0=bank.t[:, off + 64:off + 65],
                                                                    in1=snk.t[:, hq:hq + 1], op=ALU.add),
                                 reads=[bank.b, snk.b], writes=[sm.b])
                            k.op("dve", lambda en: en.reciprocal(sm.t[:, 2 + j:3 + j], sm.t[:, j:j + 1]), reads=[sm.b], writes=[sm.b])
                            k.op("dve", lambda en: en.tensor_scalar(out=of.t[:, 64 * j:64 * j + 64], in0=bank.t[:, off:off + 64],
                                                                    scalar1=sm.t[:, 2 + j:3 + j], scalar2=None, op0=ALU.mult),
                                 reads=[bank.b, sm.b], writes=[of.b])
                        k.op("pe", lambda en: en.transpose(out=ps_t.t[:, qt * 128:(qt + 1) * 128], in_=of.t[:], identity=ident.t[:]),
                             reads=[of.b, ident.b], writes=[ps_t.b], inc=(qt == 3))
                    k.op("act", lambda en: en.activation(out=ao.t[:], in_=ps_t.t[:, 0:512], func=AF.Copy), reads=[ps_t.b], writes=[ao.b])
                    k.dma("sp", [(AOT[hp * 128:(hp + 1) * 128, qb * 512:(qb + 1) * 512], ao.t[:])],
                          reads=[ao.b], writes=[AOT_b], kbuf=ao.b)
    k.barrier()


def layer_norm_tile(k, pre, st, mv, sm, gB, bB):
    for j in range(8):
        k.op("dve", lambda en: en.bn_stats(out=st.t[:, j * 6:(j + 1) * 6], in_=pre.t[:, j * 512:(j + 1) * 512]),
             reads=[pre.b], writes=[st.b], inc=(j == 7))
    k.op("dve", lambda en: en.bn_aggr(out=mv.t[:, 0:2], in_=st.t[:, 0:48]), reads=[st.b], writes=[mv.b])
    k.op("dve", lambda en: en.tensor_scalar(out=sm.t[:, 0:1], in0=mv.t[:, 1:2], scalar1=LN_EPS, scalar2=None, op0=ALU.add),
         reads=[mv.b], writes=[sm.b])
    k.op("act", lambda en: en.activation(out=sm.t[:, 1:2], in_=sm.t[:, 0:1], func=AF.Ln), reads=[sm.b], writes=[sm.b])
    k.op("act", lambda en: en.activation(out=sm.t[:, 2:3], in_=sm.t[:, 1:2], func=AF.Exp, scale=-0.5), reads=[sm.b], writes=[sm.b])
    k.op("dve", lambda en: en.tensor_scalar(out=pre.t[:], in0=pre.t[:], scalar1=mv.t[:, 0:1], scalar2=sm.t[:, 2:3],
                                            op0=ALU.subtract, op1=ALU.mult),
         reads=[pre.b, mv.b, sm.b], writes=[pre.b])
    k.op("pool", lambda en: en.tensor_tensor(out=pre.t[:], in0=pre.t[:], in1=gB.t[:], op=ALU.mult),
         reads=[pre.b, gB.b], writes=[pre.b])
    k.op("dve", lambda en: en.tensor_tensor(out=pre.t[:], in0=pre.t[:], in1=bB.t[:], op=ALU.add),
         reads=[pre.b, bB.b], writes=[pre.b])


def transpose_tile_to_bf16(k, src, xTb, col0, identb, ps_tr, hi, lo=None, xTlo=None):
    k.op("act", lambda en: en.activation(out=hi.t[:], in_=src.t[:], func=AF.Copy), reads=[src.b], writes=[hi.b])
    jobs = [(hi, xTb, col0)]
    if lo is not None:
        k.op("dve", lambda en: en.tensor_tensor(out=lo.t[:], in0=src.t[:], in1=hi.t[:], op=ALU.subtract),
             reads=[src.b, hi.b], writes=[lo.b])
        jobs.append((lo, xTlo, 0))
    gi = 0
    for (s_, dst, c0) in jobs:
        for g in range(8):
            p = ps_tr[gi % 2]
            gi += 1
            for j in range(4):
                kc = g * 4 + j
                k.op("pe", lambda en: en.transpose(out=p.t[:, j * 128:(j + 1) * 128], in_=s_.t[:, kc * 128:(kc + 1) * 128],
                                                   identity=identb.t[:]),
                     reads=[s_.b, identb.b], writes=[p.b], inc=(j == 3))
            k.op("act", lambda en: en.activation(out=dst.t[:, g * 4:(g + 1) * 4, c0:c0 + 128],
                                                 in_=p.t[:, 0:512].rearrange("p (a b) -> p a b", b=128), func=AF.Copy),
                 reads=[p.b], writes=[dst.b])


def stage_outproj(k, C, AOT, AOT_b, w_o, xres, ln_g, ln_b, router_w, router_b, win_b,
                  X1, X1_b, X1T, X1T_b, CMB, CMB_b):
    TB = 256
    with ExitStack() as es:
        identb = k.tile(es, "identb", [128, 128], BF16)
        k.dma("sp", [(identb.t[:], C["ident_bf"][:, :])], reads=[win_b], writes=[identb.b], kbuf=identb.b)
        gB = k.tile(es, "gB", [128, D], F32)
        bB = k.tile(es, "bB", [128, D], F32)
        k.dma("sp", [(gB.t[:], ln_g[None, :].to_broadcast([128, D]))], reads=[win_b], writes=[gB.b], kbuf=gB.b)
        k.dma("sp", [(bB.t[:], ln_b[None, :].to_broadcast([128, D]))], reads=[win_b], writes=[bB.b], kbuf=bB.b)
        rw = k.tile(es, "rw", [128, 32, 32], F32)
        k.dma("sp", [(rw.t[:], router_w.rearrange("(kc p) e -> p kc e", p=128))], reads=[win_b], writes=[rw.b], kbuf=rw.b)
        rbB = k.tile(es, "rbB", [128, 32], F32)
        k.dma("sp", [(rbB.t[:], router_b[None, :].to_broadcast([128, 32]))], reads=[win_b], writes=[rbB.b], kbuf=rbB.b)
        rwh = k.tile(es, "rwh", [128, 32, 32], BF16)
        rwl = k.tile(es, "rwl", [128, 32, 32], BF16)
        k.op("act", lambda en: en.activation(out=rwh.t[:], in_=rw.t[:], func=AF.Copy), reads=[rw.b], writes=[rwh.b])
        k.op("dve", lambda en: en.tensor_tensor(out=rwl.t[:], in0=rw.t[:], in1=rwh.t[:], op=ALU.subtract),
             reads=[rw.b, rwh.b], writes=[rwl.b])
        AOTb = k.tile(es, "AOTb", [128, 32, TB], BF16)
        Wo = k.tile(es, "Wo", [128, 32, 512], BF16)
        pre = [k.tile(es, "pre", [128, D], F32) for _ in range(TB // 128)]
        Phi = k.tile(es, "Phi", [128, D], BF16)
        Plo = k.tile(es, "Plo", [128, D], BF16)
        xTlo = k.tile(es, "xTlo", [128, 32, 128], BF16)
        xTb = k.tile(es, "x1Tb", [128, 32, TB], BF16)
        cmbT = k.tile(es, "cmbT", [32, TB], BF16)
        st = k.tile(es, "st", [128, 48], F32)
        mv = k.tile(es, "mv", [128, 2], F32)
        sm = k.tile(es, "smC", [128, 8], F32)
        lg = k.tile(es, "lg", [128, 32], F32)
        ex = k.tile(es, "ex", [128, 32], F32)
        mk = k.tile(es, "mk", [128, 32], F32)
        t8 = k.tile(es, "t8", [128, 8], F32)
        cb = k.tile(es, "cb", [128, 32], BF16)
        ps = [k.ptile(es, "psC", [128, 512], F32) for _ in range(3)]
        ps_tr = [k.ptile(es, "ps_tr", [128, 1024], BF16) for _ in range(2)]
        ps_lg = k.ptile(es, "ps_lg", [128, 512], F32)
        ps_ct = k.ptile(es, "ps_ct", [128, 1024], BF16)
        AOTv = AOT.rearrange("(kc p) s -> p kc s", p=128)
        wov = w_o.rearrange("(kc p) n -> p kc n", p=128)
        X1Tv = X1T.rearrange("(kc p) s -> p kc s", p=128)
        cnt = 0
        for tb in range(DBG_TB or (S // TB)):
            t0 = tb * TB
            k.dma("sp", [(AOTb.t[:], AOTv[:, :, t0:t0 + TB])], reads=[AOT_b], writes=[AOTb.b], kbuf=AOTb.b)
            for tt in range(TB // 128):
                k.dma("sp", [(pre[tt].t[:], xres[t0 + tt * 128:t0 + (tt + 1) * 128, :])], reads=[win_b], writes=[pre[tt].b],
                      kbuf=pre[tt].b)
            for cbk in range(8):
                k.dma("pool", [(Wo.t[:, j * 8:(j + 1) * 8, :], wov[:, j * 8:(j + 1) * 8, cbk * 512:(cbk + 1) * 512]) for j in range(4)],
                      reads=[win_b], writes=[Wo.b], kbuf=Wo.b)
                for tt in range(TB // 128):
                    p = ps[cnt % 3]
                    cnt += 1
                    for kc in range(32):
                        k.op("pe", lambda en: en.matmul(p.t[:], AOTb.t[:, kc, tt * 128:(tt + 1) * 128], Wo.t[:, kc, :],
                                                        start=(kc == 0), stop=(kc == 31)),
                             reads=[AOTb.b, Wo.b], writes=[p.b], inc=(kc == 31))
                    sl_ = pre[tt].t[:, cbk * 512:(cbk + 1) * 512]
                    k.op("dve", lambda en: en.scalar_tensor_tensor(out=sl_, in0=sl_, scalar=float(ALPHA), in1=p.t[:],
                                                                   op0=ALU.mult, op1=ALU.add),
                         reads=[pre[tt].b, p.b], writes=[pre[tt].b])
            for tt in range(TB // 128):
                P = pre[tt]
                layer_norm_tile(k, P, st, mv, sm, gB, bB)
                tok0 = t0 + tt * 128
                k.dma("sp", [(X1[tok0:tok0 + 128, :], P.t[:])], reads=[P.b], writes=[X1_b], kbuf=P.b)
                transpose_tile_to_bf16(k, P, xTb, tt * 128, identb, ps_tr, Phi, lo=Plo, xTlo=xTlo)
                n_ = 0
                for (use_lo, rwt) in ((False, rwh), (False, rwl), (True, rwh)):
                    for kc in range(32):
                        lhs = xTlo.t[:, kc, :] if use_lo else xTb.t[:, kc, tt * 128:(tt + 1) * 128]
                        k.op("pe", lambda en: en.matmul(ps_lg.t[:, 0:32], lhs, rwt.t[:, kc, :], start=(n_ == 0), stop=(n_ == 95)),
                             reads=[xTlo.b, xTb.b, rwt.b], writes=[ps_lg.b], inc=(n_ == 95))
                        n_ += 1
                k.op("dve", lambda en: en.tensor_tensor(out=lg.t[:], in0=ps_lg.t[:, 0:32], in1=rbB.t[:], op=ALU.add),
                     reads=[ps_lg.b, rbB.b], writes=[lg.b])
                k.op("dve", lambda en: en.max(out=t8.t[:], in_=lg.t[:]), reads=[lg.b], writes=[t8.b])
                k.op("dve", lambda en: en.tensor_scalar(out=sm.t[:, 4:5], in0=t8.t[:, 0:1], scalar1=-1.0, scalar2=None, op0=ALU.mult),
                     reads=[t8.b], writes=[sm.b])
                k.op("act", lambda en: en.activation(out=ex.t[:], in_=lg.t[:], func=AF.Exp, bias=sm.t[:, 4:5], scale=1.0),
                     reads=[lg.b, sm.b], writes=[ex.b])
                k.op("dve", lambda en: en.tensor_scalar(out=mk.t[:], in0=lg.t[:], scalar1=t8.t[:, 3:4], scalar2=None, op0=ALU.is_ge),
                     reads=[lg.b, t8.b], writes=[mk.b])
                k.op("dve", lambda en: en.tensor_tensor(out=ex.t[:], in0=ex.t[:], in1=mk.t[:], op=ALU.mult),
                     reads=[ex.b, mk.b], writes=[ex.b])
                k.op("dve", lambda en: en.reduce_sum(out=sm.t[:, 5:6], in_=ex.t[:], axis=AX.X), reads=[ex.b], writes=[sm.b])
                k.op("dve", lambda en: en.reciprocal(sm.t[:, 6:7], sm.t[:, 5:6]), reads=[sm.b], writes=[sm.b])
                k.op("dve", lambda en: en.tensor_scalar(out=cb.t[:], in0=ex.t[:], scalar1=sm.t[:, 6:7], scalar2=None, op0=ALU.mult),
                     reads=[ex.b, sm.b], writes=[cb.b])
                k.op("pe", lambda en: en.transpose(out=ps_ct.t[0:32, 0:128], in_=cb.t[:], identity=identb.t[:]),
                     reads=[cb.b, identb.b], writes=[ps_ct.b])
                k.op("act", lambda en: en.activation(out=cmbT.t[:, tt * 128:(tt + 1) * 128], in_=ps_ct.t[0:32, 0:128], func=AF.Copy),
                     reads=[ps_ct.b], writes=[cmbT.b])
            k.dma("sp", [(X1Tv[:, :, t0:t0 + TB], xTb.t[:])], reads=[xTb.b], writes=[X1T_b], kbuf=xTb.b)
            k.dma("sp", [(CMB[:, t0:t0 + TB], cmbT.t[:])], reads=[cmbT.b], writes=[CMB_b], kbuf=cmbT.b)
    k.barrier()


def stage_prestage(k, w_gate, w_up, w_down, WG, WU, WD, WS_b, win_b):
    k.nobar.add("d_" + WS_b.name)
    for e in range(w_gate.shape[0]):
        wgv = w_gate[e].rearrange("(kc p) n -> p kc n", p=128)
        wuv = w_up[e].rearrange("(kc p) n -> p kc n", p=128)
        wdv = w_down[e].rearrange("(ht p) n -> p ht n", p=128)
        for hq in range(4):
            k.dma("pool", [(WG[e, :, hq, :, :], wgv[:, :, hq * 256:(hq + 1) * 256]),
                           (WU[e, :, hq, :, :], wuv[:, :, hq * 256:(hq + 1) * 256])], reads=[win_b], writes=[WS_b], kbuf=WS_b)
        for cbk in range(8):
            k.dma("pool", [(WD[e, :, cbk, :, :], wdv[:, :, cbk * 512:(cbk + 1) * 512])], reads=[win_b], writes=[WS_b], kbuf=WS_b)


def stage_moe(k, C, X1, X1_b, X1T, X1T_b, CMB, CMB_b, w_gate, b_gate, w_up, b_up, w_down, b_down, ln_g, ln_b, win_b,
              XO, XO_b, XOT, XOT_b, WG, WU, WD, WS_b):
    TB = 512
    NT = TB // 128
    with ExitStack() as es:
        identb = k.tile(es, "identb", [128, 128], BF16)
        k.dma("sp", [(identb.t[:], C["ident_bf"][:, :])], reads=[win_b], writes=[identb.b], kbuf=identb.b)
        bgT = k.tile(es, "bgT", [128, 8, 32], F32)
        buT = k.tile(es, "buT", [128, 8, 32], F32)
        with ExitStack() as es2:
            braw = k.tile(es2, "braw", [32, 2, DH], F32)
            brh = k.tile(es2, "brh", [32, 2, DH], BF16)
            brl = k.tile(es2, "brl", [32, 2, DH], BF16)
            tlo = k.tile(es2, "tlo", [128, 256], F32)
            pb = k.ptile(es2, "pb", [128, 1024], BF16)
            k.dma("sp", [(braw.t[:, 0, :], b_gate[:, :]), (braw.t[:, 1, :], b_up[:, :])], reads=[win_b], writes=[braw.b], kbuf=braw.b)
            k.op("act", lambda en: en.activation(out=brh.t[:], in_=braw.t[:], func=AF.Copy), reads=[braw.b], writes=[brh.b])
            k.op("dve", lambda en: en.tensor_tensor(out=brl.t[:], in0=braw.t[:], in1=brh.t[:], op=ALU.subtract),
                 reads=[braw.b, brh.b], writes=[brl.b])
            for which, dst in ((0, bgT), (1, buT)):
                for part, src_ in enumerate((brh, brl)):
                    for j in range(8):
                        k.op("pe", lambda en: en.transpose(out=pb.t[:, part * 256 + j * 32:part * 256 + (j + 1) * 32],
                                                           in_=src_.t[:, which, j * 128:(j + 1) * 128], identity=identb.t[0:32, 0:32]),
                             reads=[src_.b, identb.b], writes=[pb.b], inc=(part == 1 and j == 7))
                k.op("dve", lambda en: en.tensor_copy(tlo.t[:], pb.t[:, 256:512]), reads=[pb.b], writes=[tlo.b])
                k.op("dve", lambda en: en.tensor_tensor(out=dst.t[:, :, :], in0=pb.t[:, 0:256].rearrange("p (a b) -> p a b", b=32),
                                                        in1=tlo.t[:, :].rearrange("p (a b) -> p a b", b=32), op=ALU.add),
                     reads=[pb.b, tlo.b], writes=[dst.b])
        k.barrier()
        x1Tb = k.tile(es, "x1Tb", [128, 32, TB], BF16)
        yacc = [k.tile(es, "yacc", [128, D], F32) for _ in range(NT)]
        cT = k.tile(es, "cT", [32, TB], BF16)
        X1Tv = X1T.rearrange("(kc p) s -> p kc s", p=128)
        for tb in range(DBG_TB or (S // TB)):
            t0 = tb * TB
            k.dma("sp", [(x1Tb.t[:], X1Tv[:, :, t0:t0 + TB])], reads=[X1T_b], writes=[x1Tb.b], kbuf=x1Tb.b)
            k.dma("sp", [(cT.t[:], CMB[:, t0:t0 + TB])], reads=[CMB_b], writes=[cT.b], kbuf=cT.b)
            for tt in range(NT):
                k.dma("sp", [(yacc[tt].t[:], X1[t0 + tt * 128:t0 + (tt + 1) * 128, :])], reads=[X1_b], writes=[yacc[tt].b],
                      kbuf=yacc[tt].b)
            with ExitStack() as es_i:
                bdb = k.tile(es_i, "bdb", [32, D], BF16)
                k.dma("pool", [(bdb.t[:], b_down[:, :])], reads=[win_b], writes=[bdb.b], kbuf=bdb.b)
                ps_i = [k.ptile(es_i, "ps_i", [128, 512], F32) for _ in range(3)]
                for tt in range(NT):
                    for cbk in range(8):
                        p = ps_i[(tt * 8 + cbk) % 3]
                        k.op("pe", lambda en: en.matmul(p.t[:], cT.t[:, tt * 128:(tt + 1) * 128], bdb.t[:, cbk * 512:(cbk + 1) * 512],
                                                        start=True, stop=True),
                             reads=[cT.b, bdb.b], writes=[p.b])
                        sl_ = yacc[tt].t[:, cbk * 512:(cbk + 1) * 512]
                        k.op("dve", lambda en: en.scalar_tensor_tensor(out=sl_, in0=sl_, scalar=float(ALPHA), in1=p.t[:],
                                                                       op0=ALU.mult, op1=ALU.add),
                             reads=[yacc[tt].b, p.b], writes=[yacc[tt].b])
            k.barrier()
            with ExitStack() as es3:
                Wg = [k.tile(es3, "Wg", [128, 32, 256], BF16) for _ in range(2)]
                Wu = [k.tile(es3, "Wu", [128, 32, 256], BF16) for _ in range(2)]
                Wd = [k.tile(es3, "Wd", [128, 8, 512], BF16) for _ in range(2)]
                hT = k.tile(es3, "hT", [128, 8, TB], BF16)
                cBs = [k.tile(es3, "cB", [128, TB], BF16) for _ in range(2)]
                tg = [k.tile(es3, "tg", [128, TB], F32) for _ in range(2)]
                ts_ = [k.tile(es3, "ts", [128, TB], F32) for _ in range(2)]
                tu = [k.tile(es3, "tu", [128, TB], F32) for _ in range(2)]
                ps_g = [k.ptile(es3, "ps_g", [128, 512], F32) for _ in range(2)]
                ps_u = [k.ptile(es3, "ps_u", [128, 512], F32) for _ in range(2)]
                ps_y = [k.ptile(es3, "ps_y", [128, 512], F32) for _ in range(3)]
                wi = 0
                di = 0
                ti = 0
                yi = 0
                for e in range(NE if DBG_VIRT else (DBG_NE or NE)):
                    cB = cBs[e % 2]
                    k.dma("sp", [(cB.t[:], CMB[e, t0:t0 + TB][None, :].to_broadcast([128, TB]))], reads=[CMB_b], writes=[cB.b], kbuf=cB.b)
                    ew = e % w_gate.shape[0]
                    for hq in range(4):
                        G_ = Wg[wi % 2]
                        U_ = Wu[wi % 2]
                        wi += 1
                        k.dma("sp", [(G_.t[:, :, :], WG[ew, :, hq, :, :])], reads=[WS_b], writes=[G_.b], kbuf=G_.b)
                        k.dma("sp", [(U_.t[:, :, :], WU[ew, :, hq, :, :])], reads=[WS_b], writes=[U_.b], kbuf=U_.b)
                        for ht2 in range(2):
                            ht = hq * 2 + ht2
                            pg = ps_g[ti % 2]
                            pu = ps_u[ti % 2]
                            g_ = tg[ti % 2]
                            s_ = ts_[ti % 2]
                            u_ = tu[ti % 2]
                            ti += 1
                            for kc in range(32):
                                k.op("pe", lambda en: en.matmul(pg.t[:], G_.t[:, kc, ht2 * 128:(ht2 + 1) * 128], x1Tb.t[:, kc, :],
                                                                start=(kc == 0), stop=(kc == 31)),
                                     reads=[G_.b, x1Tb.b], writes=[pg.b], inc=(kc == 31))
                            for kc in range(32):
                                k.op("pe", lambda en: en.matmul(pu.t[:], U_.t[:, kc, ht2 * 128:(ht2 + 1) * 128], x1Tb.t[:, kc, :],
                                                                start=(kc == 0), stop=(kc == 31)),
                                     reads=[U_.b, x1Tb.b], writes=[pu.b], inc=(kc == 31))
                            k.op("dve", lambda en: en.tensor_scalar(out=g_.t[:], in0=pg.t[:], scalar1=bgT.t[:, ht, e:e + 1], scalar2=7.0,
                                                                    op0=ALU.add, op1=ALU.min),
                                 reads=[pg.b, bgT.b], writes=[g_.b])
                            k.op("act", lambda en: en.activation(out=s_.t[:], in_=g_.t[:], func=AF.Sigmoid, scale=1.702),
                                 reads=[g_.b], writes=[s_.b])
                            k.op("dve", lambda en: en.tensor_scalar(out=u_.t[:], in0=pu.t[:], scalar1=buT.t[:, ht, e:e + 1], scalar2=7.0,
                                                                    op0=ALU.add, op1=ALU.min),
                                 reads=[pu.b, buT.b], writes=[u_.b])
                            k.op("pool", lambda en: en.tensor_scalar(out=u_.t[:], in0=u_.t[:], scalar1=-7.0, scalar2=1.0,
                                                                     op0=ALU.max, op1=ALU.add),
                                 reads=[u_.b], writes=[u_.b])
                            k.op("pool", lambda en: en.tensor_tensor(out=s_.t[:], in0=s_.t[:], in1=g_.t[:], op=ALU.mult),
                                 reads=[s_.b, g_.b], writes=[s_.b])
                            k.op("pool", lambda en: en.tensor_tensor(out=u_.t[:], in0=u_.t[:], in1=cB.t[:], op=ALU.mult),
                                 reads=[u_.b, cB.b], writes=[u_.b])
                            k.op("dve", lambda en: en.tensor_tensor(out=hT.t[:, ht, :], in0=s_.t[:], in1=u_.t[:], op=ALU.mult),
                                 reads=[s_.b, u_.b], writes=[hT.b])
                    for cbk in range(8):
                        Dw = Wd[di % 2]
                        di += 1
                        k.dma("sp", [(Dw.t[:], WD[ew, :, cbk, :, :])], reads=[WS_b], writes=[Dw.b], kbuf=Dw.b)
                        for tt in range(NT):
                            p = ps_y[yi % 3]
                            yi += 1
                            for ht in range(8):
                                k.op("pe", lambda en: en.matmul(p.t[:], hT.t[:, ht, tt * 128:(tt + 1) * 128], Dw.t[:, ht, :],
                                                                start=(ht == 0), stop=(ht == 7)),
                                     reads=[hT.b, Dw.b], writes=[p.b], inc=(ht == 7))
                            sl_ = yacc[tt].t[:, cbk * 512:(cbk + 1) * 512]
                            k.op("dve", lambda en: en.tensor_tensor(out=sl_, in0=sl_, in1=p.t[:], op=ALU.add),
                                 reads=[yacc[tt].b, p.b], writes=[yacc[tt].b])
            k.barrier()
            with ExitStack() as es4:
                gB = k.tile(es4, "gB2", [128, D], F32)
                bB = k.tile(es4, "bB2", [128, D], F32)
                k.dma("sp", [(gB.t[:], ln_g[None, :].to_broadcast([128, D]))], reads=[win_b], writes=[gB.b], kbuf=gB.b)
                k.dma("sp", [(bB.t[:], ln_b[None, :].to_broadcast([128, D]))], reads=[win_b], writes=[bB.b], kbuf=bB.b)
                st = k.tile(es4, "st", [128, 48], F32)
                mv = k.tile(es4, "mv", [128, 2], F32)
                sm = k.tile(es4, "smD", [128, 8], F32)
                xoT = k.tile(es4, "xoT", [128, 32, TB], BF16) if XOT is not None else None
                Phi = k.tile(es4, "Phi", [128, D], BF16)
                ps_tr = [k.ptile(es4, "ps_tr", [128, 1024], BF16) for _ in range(2)]
                for tt in range(NT):
                    P = yacc[tt]
                    layer_norm_tile(k, P, st, mv, sm, gB, bB)
                    tok0 = t0 + tt * 128
                    k.dma("sp", [(XO[tok0:tok0 + 128, :], P.t[:])], reads=[P.b], writes=[XO_b], kbuf=P.b)
                    if XOT is not None:
                        transpose_tile_to_bf16(k, P, xoT, tt * 128, identb, ps_tr, Phi)
                if XOT is not None:
                    XOTv = XOT.rearrange("(kc p) s -> p kc s", p=128)
                    k.dma("sp", [(XOTv[:, :, t0:t0 + TB], xoT.t[:])], reads=[xoT.b], writes=[XOT_b], kbuf=xoT.b)
            k.barrier()
    k.barrier()


CONST_SPECS = {
    "dtab": ([128, 1024], F32), "mcausal": ([128, 1024], F32), "mband": ([128, 1024], F32),
    "ident_bf": ([128, 128], BF16), "ident_f": ([128, 128], F32),
    "onehot16": ([16, 2048], BF16), "onehot32": ([32, 4096], BF16),
    "pastbias": ([128, 512], F32), "pastmask": ([128, 512], F32), "ownmask": ([128, 512], F32),
}


def make_consts():
    i = np.arange(128)[:, None]
    c = np.arange(1024)[None, :] - 384
    d = (c - i).astype(np.float32)
    out = {"dtab": d,
           "mcausal": np.where(c >= i, 0.0, NEG).astype(np.float32),
           "mband": np.where((c - i >= 0) & (c - i < 128), 0.0, NEG).astype(np.float32),
           "ident_bf": np.eye(128, dtype=np.float32).astype(ml_dtypes.bfloat16),
           "ident_f": np.eye(128, dtype=np.float32)}
    oh16 = np.zeros((16, 16, 128), np.float32)
    for p in range(16):
        oh16[p, p, :] = 1.0
    out["onehot16"] = oh16.reshape(16, 2048).astype(ml_dtypes.bfloat16)
    oh32 = np.zeros((32, 32, 128), np.float32)
    for p in range(32):
        oh32[p, p, :] = 1.0
    out["onehot32"] = oh32.reshape(32, 4096).astype(ml_dtypes.bfloat16)
    t = np.arange(32)[:, None]
    n = np.arange(16)[None, :]
    past = (n < t // 2)
    own = (n == t // 2)
    out["pastbias"] = np.broadcast_to(np.where(past, 0.0, NEG).astype(np.float32).reshape(1, 512), (128, 512)).copy()
    out["pastmask"] = np.broadcast_to(past.astype(np.float32).reshape(1, 512), (128, 512)).copy()
    out["ownmask"] = np.broadcast_to(own.astype(np.float32).reshape(1, 512), (128, 512)).copy()
    return out


LAYER_KEYS = ["router_w", "router_b", "w_gate", "b_gate", "w_up", "b_up", "w_down", "b_down",
              "ln1_g", "ln1_b", "ln2_g", "ln2_b", "w_in", "w_o"]
IN_SHAPES = {
    "xT": [D, S], "x": [S, D],
    "l0_w_in": [D, 12288], "l0_lambda_q1": [64], "l0_lambda_k1": [64], "l0_lambda_q2": [64], "l0_lambda_k2": [64],
    "l0_subln_g": [128], "l0_w_o": [D, D], "l1_w_in": [D, 5120], "l1_sinks": [64], "l1_w_o": [D, D],
}
for _l in ("l0", "l1"):
    IN_SHAPES.update({f"{_l}_ln1_g": [D], f"{_l}_ln1_b": [D], f"{_l}_ln2_g": [D], f"{_l}_ln2_b": [D],
                      f"{_l}_router_w": [D, NE], f"{_l}_router_b": [NE],
                      f"{_l}_w_gate": [NE, D, DH], f"{_l}_b_gate": [NE, DH], f"{_l}_w_up": [NE, D, DH], f"{_l}_b_up": [NE, DH],
                      f"{_l}_w_down": [NE, DH, D], f"{_l}_b_down": [NE, D]})


def build_program(stages=("A0", "P0", "B0", "C0", "D0", "A1", "P1", "B1", "C1", "D1"), debug_out=(), need=None):
    nc = bass.Bass("TRN2", target_bir_lowering=False)
    I = {}
    names = list(IN_SHAPES) if need is None else list(need)
    for nm in names:
        shp = list(IN_SHAPES[nm])
        if DBG_NE and len(shp) == 3:
            shp[0] = DBG_NE
        I[nm] = nc.dram_tensor(nm, shp, F32, kind="ExternalInput").ap()
    C = {}
    for nm, (shp, dt) in CONST_SPECS.items():
        C[nm] = nc.dram_tensor("c_" + nm, shp, dt, kind="ExternalInput").ap()

    def dram(nm, shp, dt, out=False):
        kind = "ExternalOutput" if (out or nm in debug_out) else "Internal"
        return nc.dram_tensor(nm, shp, dt, kind=kind).ap()

    PT0 = dram("PT0", [8192, S], BF16)
    VN0 = dram("VN0", [S, 4096], BF16)
    AOT = dram("AOT", [4096, S], BF16)
    X1 = dram("X1", [S, D], F32)
    X1T = dram("X1T", [D, S], BF16)
    CMB = dram("CMB", [NE, S], BF16)
    X2 = dram("X2", [S, D], F32)
    X2T = dram("X2T", [D, S], BF16)
    PT1 = dram("PT1", [4608, S], BF16)
    VN1 = dram("VN1", [S, 512], BF16)
    AOT1 = dram("AOT1", [4096, S], BF16)
    X3 = dram("X3", [S, D], F32)
    X3T = dram("X3T", [D, S], BF16)
    CMB1 = dram("CMB1", [NE, S], BF16)
    OUT = dram("out", [S, D], F32, out=True)
    WS = []
    for l in range(2):
        WS.append((dram(f"WG{l}", [NE, 128, 4, 32, 256], BF16), dram(f"WU{l}", [NE, 128, 4, 32, 256], BF16),
                   dram(f"WD{l}", [NE, 128, 8, 8, 512], BF16), Buf(f"WS{l}", nowaw=True, sticky=True)))
    bufs = {nm: Buf(nm, nowaw=True) for nm in ["PT0", "VN0", "AOT", "X1", "X1T", "CMB", "X2", "X2T", "PT1", "VN1", "AOT1",
                                              "X3", "X3T", "CMB1", "OUT", "win"]}
    win_b = bufs["win"]
    with ExitStack() as es:
        k = KB(nc, es)
        globals()["_LASTK"] = k
        es.enter_context(nc.Block())
        if "A0" in stages:
            blocks = []
            for j in range(24):
                c0 = 512 * j
                if j < 8:
                    blocks.append((c0, "T", 512 * j, 0.125 if j < 4 else 1.0))
                elif j < 12:
                    blocks.append((c0, "N", 512 * (j - 8), 1.0))
                elif j < 20:
                    blocks.append((c0, "T", 4096 + 512 * (j - 12), 128.0 ** -0.5 if j < 16 else 1.0))
                else:
                    blocks.append((c0, "N", 2048 + 512 * (j - 20), 1.0))
            stage_inproj(k, I["xT"], win_b, I["l0_w_in"], win_b, blocks, PT0, bufs["PT0"], VN0, bufs["VN0"])
        if "P0" in stages:
            stage_prestage(k, I["l0_w_gate"], I["l0_w_up"], I["l0_w_down"], *WS[0], win_b)
        if "B0" in stages:
            stage_attn_l0(k, C, PT0, bufs["PT0"], VN0, bufs["VN0"], AOT, bufs["AOT"],
                          [I["l0_lambda_q1"], I["l0_lambda_k1"], I["l0_lambda_q2"], I["l0_lambda_k2"]], I["l0_subln_g"], win_b)
        if "C0" in stages:
            stage_outproj(k, C, AOT, bufs["AOT"], I["l0_w_o"], I["x"], I["l0_ln1_g"], I["l0_ln1_b"], I["l0_router_w"],
                          I["l0_router_b"], win_b, X1, bufs["X1"], X1T, bufs["X1T"], CMB, bufs["CMB"])
        if "D0" in stages:
            stage_moe(k, C, X1, bufs["X1"], X1T, bufs["X1T"], CMB, bufs["CMB"], I["l0_w_gate"], I["l0_b_gate"], I["l0_w_up"],
                      I["l0_b_up"], I["l0_w_down"], I["l0_b_down"], I["l0_ln2_g"], I["l0_ln2_b"], win_b,
                      X2, bufs["X2"], X2T, bufs["X2T"], *WS[0])
        if "A1" in stages:
            blocks = [(512 * j, "T", 512 * j, 0.125) for j in range(8)] + [(4096, "T", 4096, 1.0), (4608, "N", 0, 1.0)]
            stage_inproj(k, X2T, bufs["X2T"], I["l1_w_in"], win_b, blocks, PT1, bufs["PT1"], VN1, bufs["VN1"])
        if "P1" in stages:
            stage_prestage(k, I["l1_w_gate"], I["l1_w_up"], I["l1_w_down"], *WS[1], win_b)
        if "B1" in stages:
            stage_attn_l1(k, C, PT1, bufs["PT1"], VN1, bufs["VN1"], AOT1, bufs["AOT1"], I["l1_sinks"], win_b)
        if "C1" in stages:
            stage_outproj(k, C, AOT1, bufs["AOT1"], I["l1_w_o"], X2, I["l1_ln1_g"], I["l1_ln1_b"], I["l1_router_w"],
                          I["l1_router_b"], bufs["X2"], X3, bufs["X3"], X3T, bufs["X3T"], CMB1, bufs["CMB1"])
        if "D1" in stages:
            stage_moe(k, C, X3, bufs["X3"], X3T, bufs["X3T"], CMB1, bufs["CMB1"], I["l1_w_gate"], I["l1_b_gate"], I["l1_w_up"],
                      I["l1_b_up"], I["l1_w_down"], I["l1_b_down"], I["l1_ln2_g"], I["l1_ln2_b"], win_b,
                      OUT, bufs["OUT"], None, None, *WS[1])
        k.final_wait()
    return nc, names


_CONSTS = None


def kernel(**inputs):
    global _CONSTS
    if _CONSTS is None:
        _CONSTS = make_consts()
    nc, names = build_program()
    x = np.asarray(inputs["x"], dtype=np.float32)
    shared = {}
    for nm in names:
        if nm not in ("x", "xT"):
            shared[nm] = np.ascontiguousarray(np.asarray(inputs[nm], dtype=np.float32))
    in_maps = []
    for b in range(NCORES):
        m = dict(shared)
        m["xT"] = np.ascontiguousarray(x[b].T)
        m["x"] = np.ascontiguousarray(x[b])
        for nm, v in _CONSTS.items():
            m["c_" + nm] = v
        in_maps.append(m)
    res = run_bass_kernel_spmd(nc, in_maps, core_ids=list(range(NCORES)))
    return np.stack([np.asarray(res.results[b]["out"], dtype=np.float32) for b in range(NCORES)], axis=0)
```
